# Optimizing a Trainium2 kernel written in Bass

```python
import jax, jax.numpy as jnp
from jax import lax
import numpy as np

D_MODEL = 2048
BATCH = 1
SEQ = 16384
DEPTH = 1

D_MIX = D_MODEL
D_GMLP = D_MIX // 2
GMLP_GROUPS = 8
GMLP_GROUP_DIM = D_GMLP // GMLP_GROUPS
GMLP_CHUNK = 128
D_MLSTM = D_MIX - D_GMLP
MLSTM_HEADS = 4
MLSTM_HEAD_DIM = D_MLSTM // MLSTM_HEADS
MLSTM_CHUNK = 128
CONV_WIDTH = 4
PEER_HEADS = 8
PEER_N_KEYS = 128
PEER_N_EXPERTS = PEER_N_KEYS * PEER_N_KEYS
PEER_QUERY_DIM = 256
PEER_HALF_DIM = PEER_QUERY_DIM // 2
PEER_TOPK = 16
PEER_TOKEN_BLOCK = 128
N_MOD = 6
EPS = 1e-6
IN_COLS = 2 * D_GMLP + 4 * D_MLSTM + 2 * MLSTM_HEADS
IN_SPLITS = (D_GMLP, 2 * D_GMLP, 2 * D_GMLP + 2 * D_MLSTM,
             2 * D_GMLP + 3 * D_MLSTM, 2 * D_GMLP + 4 * D_MLSTM)

kernel_name = "hybrid_gmlp_mlstm_peer_block"


def rms_norm(x, gain):
    xf = x.astype(jnp.float32)
    y = xf * lax.rsqrt(jnp.mean(xf * xf, axis=-1, keepdims=True) + EPS)
    return (y * gain.astype(jnp.float32)).astype(x.dtype)


def causal_depthwise_conv(x, w, b):
    y = lax.conv_general_dilated(
        x, w[:, None, :].astype(x.dtype), window_strides=(1,),
        padding=[(CONV_WIDTH - 1, 0)], dimension_numbers=("NWC", "WIO", "NWC"),
        feature_group_count=x.shape[-1])
    return y + b


def gmlp_spatial_gating(u, v, norm_g, w_spatial, b_spatial):
    B, S, _ = u.shape
    nc = S // GMLP_CHUNK
    u = u.reshape(B, nc, GMLP_CHUNK, GMLP_GROUPS, GMLP_GROUP_DIM)
    v = rms_norm(v.reshape(B, nc, GMLP_CHUNK, GMLP_GROUPS, GMLP_GROUP_DIM), norm_g)
    pos = jnp.arange(GMLP_CHUNK)
    causal = pos[:, None] >= pos[None, :]
    w = jnp.where(causal, w_spatial, 0)
    mixed = jnp.einsum("gts,bnsgc->bntgc", w, v) + b_spatial.T[None, None, :, :, None]
    return (u * mixed).reshape(B, S, D_GMLP)


def mlstm_chunkwise(q, k, v, i_pre, log_f):
    out_dtype = v.dtype
    q, k, v = (t.astype(jnp.float32) for t in (q, k, v))
    i_pre, log_f = i_pre.astype(jnp.float32), log_f.astype(jnp.float32)
    B, H, S, d = q.shape
    L = MLSTM_CHUNK
    nc = S // L
    q = q * (d ** -0.5)

    def to_chunks(t):
        return jnp.moveaxis(t.reshape(B, H, nc, L, *t.shape[3:]), 2, 0)

    xs = tuple(to_chunks(t) for t in (q, k, v, i_pre, log_f))
    pos = jnp.arange(L)
    causal = pos[:, None] >= pos[None, :]

    def step(carry, inp):
        C, n, m = carry
        qb, kb, vb, ib, fb = inp
        b = jnp.cumsum(fb, axis=-1)
        log_d = jnp.where(causal, b[..., :, None] - b[..., None, :] + ib[..., None, :], -jnp.inf)
        a = b + m[..., None]
        m_comb = jnp.maximum(a, jnp.max(log_d, axis=-1))
        w_intra = jnp.exp(log_d - m_comb[..., None])
        w_inter = jnp.exp(a - m_comb)
        s = jnp.einsum("bhtd,bhsd->bhts", qb, kb) * w_intra
        num = (jnp.einsum("bhts,bhsd->bhtd", s, vb)
               + w_inter[..., None] * jnp.einsum("bhtd,bhde->bhte", qb, C))
        den = jnp.sum(s, axis=-1) + w_inter * jnp.einsum("bhtd,bhd->bht", qb, n)
        h = num / jnp.maximum(jnp.abs(den), jnp.exp(-m_comb))[..., None]
        b_last = b[..., -1]
        log_w = b_last[..., None] - b + ib
        m_new = jnp.maximum(b_last + m, jnp.max(log_w, axis=-1))
        w_state = jnp.exp(log_w - m_new[..., None])
        decay = jnp.exp(b_last + m - m_new)
        C_new = decay[..., None, None] * C + jnp.einsum("bhs,bhsd,bhse->bhde", w_state, kb, vb)
        n_new = decay[..., None] * n + jnp.einsum("bhs,bhsd->bhd", w_state, kb)
        return (C_new, n_new, m_new), h

    init = (jnp.zeros((B, H, d, d), jnp.float32), jnp.zeros((B, H, d), jnp.float32),
            jnp.zeros((B, H), jnp.float32))
    _, h = lax.scan(step, init, xs)
    return jnp.moveaxis(h, 0, 2).reshape(B, H, S, d).astype(out_dtype)


def peer_ffn(h, w_query, sub_keys_1, sub_keys_2, expert_down, expert_up):
    B, S, D = h.shape
    T = B * S
    x = h.reshape(T, D)
    q = (x @ w_query).reshape(T, PEER_HEADS, 2, PEER_HALF_DIM)
    s1 = jnp.einsum("thc,kc->thk", q[:, :, 0], sub_keys_1)
    s2 = jnp.einsum("thc,kc->thk", q[:, :, 1], sub_keys_2)
    v1, i1 = lax.top_k(s1, PEER_TOPK)
    v2, i2 = lax.top_k(s2, PEER_TOPK)
    cand_s = (v1[..., :, None] + v2[..., None, :]).reshape(T, PEER_HEADS, PEER_TOPK * PEER_TOPK)
    cand_i = (i1[..., :, None] * PEER_N_KEYS + i2[..., None, :]).reshape(T, PEER_HEADS, PEER_TOPK * PEER_TOPK)
    top_s, top_pos = lax.top_k(cand_s, PEER_TOPK)
    idx = jnp.take_along_axis(cand_i, top_pos, axis=-1)
    gates = jax.nn.softmax(top_s.astype(jnp.float32), axis=-1).astype(x.dtype)

    nb = T // PEER_TOKEN_BLOCK

    def expert_block(args):
        xb, ib, gb = args
        u = expert_down[ib]
        act = jax.nn.gelu(jnp.einsum("td,thkd->thk", xb, u), approximate=False)
        return jnp.einsum("thk,thkd->td", gb * act, expert_up[ib])

    y = lax.map(expert_block, (x.reshape(nb, PEER_TOKEN_BLOCK, D),
                               idx.reshape(nb, PEER_TOKEN_BLOCK, PEER_HEADS, PEER_TOPK),
                               gates.reshape(nb, PEER_TOKEN_BLOCK, PEER_HEADS, PEER_TOPK)))
    return y.reshape(B, S, D)


def setup_inputs(seed: int = 0) -> dict:
    key = jax.random.key(seed)
    ks = jax.random.split(key, 24)
    nrm = jax.random.normal
    D, H, dh = D_MODEL, MLSTM_HEADS, MLSTM_HEAD_DIM
    b_gates = jnp.concatenate(
        [0.1 * nrm(ks[6], (DEPTH, H)),
         jnp.linspace(3.0, 6.0, H)[None, :] + 0.1 * nrm(ks[7], (DEPTH, H))], axis=-1)
    return {
        "x": nrm(ks[0], (BATCH, SEQ, D)),
        "c": nrm(ks[1], (BATCH, D)),
        "ada_w": nrm(ks[2], (DEPTH, D, N_MOD * D)) * (0.5 * D ** -0.5),
        "ada_b": 0.02 * nrm(ks[3], (DEPTH, N_MOD * D)),
        "norm_mix_g": 1.0 + 0.02 * nrm(ks[4], (DEPTH, D)),
        "w_in": nrm(ks[5], (DEPTH, D, IN_COLS)) * D ** -0.5,
        "b_gates": b_gates,
        "conv_w": nrm(ks[8], (DEPTH, CONV_WIDTH, 2 * D_MLSTM)) * CONV_WIDTH ** -0.5,
        "conv_b": 0.02 * nrm(ks[9], (DEPTH, 2 * D_MLSTM)),
        "gmlp_norm_g": 1.0 + 0.02 * nrm(ks[10], (DEPTH, GMLP_GROUPS, GMLP_GROUP_DIM)),
        "gmlp_w_spatial": nrm(ks[11], (DEPTH, GMLP_GROUPS, GMLP_CHUNK, GMLP_CHUNK)) * (0.5 * GMLP_CHUNK ** -0.5),
        "gmlp_b_spatial": 1.0 + 0.02 * nrm(ks[12], (DEPTH, GMLP_GROUPS, GMLP_CHUNK)),
        "mlstm_norm_g": 1.0 + 0.02 * nrm(ks[13], (DEPTH, H, dh)),
        "w_out": nrm(ks[14], (DEPTH, D_MIX, D)) * D_MIX ** -0.5,
        "norm_ffn_g": 1.0 + 0.02 * nrm(ks[15], (DEPTH, D)),
        "peer_w_query": nrm(ks[16], (DEPTH, D, PEER_HEADS * PEER_QUERY_DIM)) * D ** -0.5,
        "peer_sub_keys_1": nrm(ks[17], (DEPTH, PEER_N_KEYS, PEER_HALF_DIM)) * PEER_HALF_DIM ** -0.5,
        "peer_sub_keys_2": nrm(ks[18], (DEPTH, PEER_N_KEYS, PEER_HALF_DIM)) * PEER_HALF_DIM ** -0.5,
        "peer_expert_down": nrm(ks[19], (DEPTH, PEER_N_EXPERTS, D)) * D ** -0.5,
        "peer_expert_up": nrm(ks[20], (DEPTH, PEER_N_EXPERTS, D)) * PEER_HEADS ** -0.5,
        "final_norm_g": 1.0 + 0.02 * nrm(ks[21], (D,)),
    }


def reference(x, c, ada_w, ada_b, norm_mix_g, w_in, b_gates, conv_w, conv_b, gmlp_norm_g,
              gmlp_w_spatial, gmlp_b_spatial, mlstm_norm_g, w_out, norm_ffn_g, peer_w_query,
              peer_sub_keys_1, peer_sub_keys_2, peer_expert_down, peer_expert_up, final_norm_g):
    B, S, _ = x.shape
    H, dh = MLSTM_HEADS, MLSTM_HEAD_DIM

    def heads(t):
        return t.reshape(B, S, H, dh).transpose(0, 2, 1, 3)

    for l in range(DEPTH):
        mod = jax.nn.silu(c) @ ada_w[l] + ada_b[l]
        shift_m, scale_m, gate_m, shift_f, scale_f, gate_f = jnp.split(mod[:, None, :], N_MOD, axis=-1)

        h = rms_norm(x, norm_mix_g[l]) * (1 + scale_m) + shift_m
        proj = h @ w_in[l]
        u_g, v_g, qk_m, v_m, o_m, if_m = jnp.split(proj, IN_SPLITS, axis=-1)

        y_g = gmlp_spatial_gating(jax.nn.gelu(u_g, approximate=False),
                                  jax.nn.gelu(v_g, approximate=False),
                                  gmlp_norm_g[l], gmlp_w_spatial[l], gmlp_b_spatial[l])

        qk = jax.nn.silu(causal_depthwise_conv(qk_m, conv_w[l], conv_b[l]))
        q_m, k_m = jnp.split(qk, 2, axis=-1)
        i_pre, f_pre = jnp.split(if_m + b_gates[l], 2, axis=-1)
        log_f = jax.nn.log_sigmoid(f_pre.astype(jnp.float32))
        h_m = mlstm_chunkwise(heads(q_m), heads(k_m), heads(v_m),
                              i_pre.transpose(0, 2, 1), log_f.transpose(0, 2, 1))
        h_m = rms_norm(h_m.transpose(0, 2, 1, 3), mlstm_norm_g[l]) * jax.nn.sigmoid(o_m).reshape(B, S, H, dh)
        y_m = h_m.reshape(B, S, D_MLSTM)

        mix = jnp.concatenate([y_g, y_m], axis=-1) @ w_out[l]
        x = x + gate_m * mix

        h = rms_norm(x, norm_ffn_g[l]) * (1 + scale_f) + shift_f
        x = x + gate_f * peer_ffn(h, peer_w_query[l], peer_sub_keys_1[l], peer_sub_keys_2[l],
                                  peer_expert_down[l], peer_expert_up[l])

    return rms_norm(x, final_norm_g)
```

```python
import numpy as np
from contextlib import ExitStack
import concourse.bass as bass
import concourse.mybir as mybir
from concourse.bass_utils import run_bass_kernel_spmd

F32 = mybir.dt.float32
BF16 = mybir.dt.bfloat16
I32 = mybir.dt.int32
ALU = mybir.AluOpType
AF = mybir.ActivationFunctionType

NCORES = 8
D = 2048
TOK = 2048
NST = 4
SN = 512
EPS = 1e-6
IN_COLS = 6152
SAME_ENG_SYNC = True


class Op:
    __slots__ = ("eng", "fn", "dma", "chan", "deps", "needed", "inc_idx", "dval", "prev_dval", "cc")


class Sched:
    ENGS = ("pe", "dve", "act", "pool", "sp")

    def __init__(self, nc):
        self.nc = nc
        self.ops = []
        self.last_w = {}
        self.readers = {}
        self.chan_cnt = {}
        self.chan_last = {}

    def barrier(self):
        lasts = {}
        for op in self.ops:
            if not op.dma and op.fn is not None:
                lasts[op.eng] = op
        deps = set(lasts.values()) | set(self.chan_last.values())
        for e in self.ENGS:
            op = self.add(e, None)
            op.deps = set(deps)

    def add(self, eng, fn, r=(), w=(), chan=None, cc=False):
        op = Op()
        op.eng, op.fn, op.chan, op.dma = eng, fn, chan, chan is not None
        op.cc = cc
        op.needed = False
        op.inc_idx = None
        op.dval = None
        op.prev_dval = None
        deps = set()
        for res in r:
            lw = self.last_w.get(res)
            if lw is not None:
                deps.add(lw)
        for res in w:
            lw = self.last_w.get(res)
            if lw is not None:
                deps.add(lw)
            for rd in self.readers.get(res, ()):
                deps.add(rd)
        op.deps = deps
        for res in r:
            self.readers.setdefault(res, []).append(op)
        for res in w:
            self.last_w[res] = op
            self.readers[res] = []
        if op.dma:
            c = self.chan_cnt.get(chan, 0)
            step = 1 if cc else 16
            op.prev_dval = c
            op.dval = c + step
            self.chan_cnt[chan] = c + step
            self.chan_last[chan] = op
        self.ops.append(op)
        return op

    @staticmethod
    def _skip(op, d):
        if d.dma:
            return False
        if d.eng == op.eng and not op.dma:
            if d.eng == "pe" or not SAME_ENG_SYNC:
                return True
        return False

    def emit(self, block):
        nc = self.nc
        for op in self.ops:
            for d in op.deps:
                if d.dma or self._skip(op, d):
                    continue
                d.needed = True
        cnt = {e: 0 for e in self.ENGS}
        for op in self.ops:
            if op.dma:
                continue
            if op.needed:
                cnt[op.eng] += 1
                op.inc_idx = cnt[op.eng]
        esem = {e: nc.alloc_semaphore("es_" + e) for e in self.ENGS}
        csem = {c: nc.alloc_semaphore("cs_%d" % i) for i, c in enumerate(self.chan_cnt)}
        per_eng = {e: [op for op in self.ops if op.eng == e] for e in self.ENGS}
        skip = self._skip

        def stream(e):
            def body(eo):
                waited = {}

                def wait(key, sem, val):
                    if val <= 0 or waited.get(key, 0) >= val:
                        return
                    waited[key] = val
                    eo.wait_ge(sem, val)

                for op in per_eng[e]:
                    for d in op.deps:
                        if d.dma:
                            wait(("c", d.chan), csem[d.chan], d.dval)
                        elif not skip(op, d):
                            wait(("e", d.eng), esem[d.eng], d.inc_idx)
                    if op.dma:
                        wait(("c", op.chan), csem[op.chan], op.prev_dval)
                    if op.fn is None:
                        continue
                    ins = op.fn(eo)
                    if op.cc:
                        ins.then_inc(csem[op.chan])
                    elif op.dma:
                        ins.then_inc(csem[op.chan], 16)
                    elif op.needed:
                        ins.then_inc(esem[e], 1)
                if e == "sp":
                    for c, v in self.chan_cnt.items():
                        wait(("c", c), csem[c], v)
            return body

        block.tensor(stream("pe"))
        block.vector(stream("dve"))
        block.scalar(stream("act"))
        block.gpsimd(stream("pool"))
        block.sync(stream("sp"))

    def dma(self, out, in_, r, w, chan, eng="sp", **kw):
        return self.add(eng, lambda e: e.dma_start(out=out, in_=in_, **kw), r=r, w=w, chan=chan)

    def mm(self, out, lhsT, rhs, start=True, stop=True, r=(), w=(), skip=False):
        if skip:
            return self.add("pe", lambda e: e.matmul(out, lhsT, rhs, start=start, stop=stop, skip_group_check=True), r=r, w=w)
        return self.add("pe", lambda e: e.matmul(out, lhsT, rhs, start=start, stop=stop), r=r, w=w)

    def tr(self, out, in_, ident, r=(), w=()):
        return self.add("pe", lambda e: e.transpose(out, in_, ident), r=r, w=w)

    def actv(self, out, in_, func, bias=None, scale=None, accum_out=None, r=(), w=(), eng="act"):
        kw = {}
        if bias is not None:
            kw["bias"] = bias
        if scale is not None:
            kw["scale"] = scale
        if accum_out is not None:
            kw["accum_out"] = accum_out
        return self.add(eng, lambda e: e.activation(out, in_, func, **kw), r=r, w=w)

    def tt(self, out, in0, in1, op, r=(), w=(), eng="dve"):
        return self.add(eng, lambda e: e.tensor_tensor(out, in0, in1, op), r=r, w=w)

    def ts(self, out, in0, s1, s2, op0, op1=None, accum_out=None, r=(), w=(), eng="dve"):
        kw = {}
        if op1 is not None:
            kw["op1"] = op1
        if accum_out is not None:
            kw["accum_out"] = accum_out
        return self.add(eng, lambda e: e.tensor_scalar(out, in0, s1, s2, op0, **kw), r=r, w=w)

    def stt(self, out, in0, scalar, in1, op0, op1, accum_out=None, r=(), w=(), eng="dve"):
        kw = {}
        if accum_out is not None:
            kw["accum_out"] = accum_out
        return self.add(eng, lambda e: e.scalar_tensor_tensor(out, in0, scalar, in1, op0, op1, **kw), r=r, w=w)

    def copy(self, out, in_, r=(), w=(), eng="dve"):
        if eng == "act":
            return self.add(eng, lambda e: e.activation(out, in_, AF.Copy), r=r, w=w)
        return self.add(eng, lambda e: e.tensor_copy(out, in_), r=r, w=w)

    def memset(self, ap, val, w=(), eng="dve"):
        return self.add(eng, lambda e: e.memset(ap, val), r=(), w=w)

    def recip(self, out, in_, r=(), w=()):
        return self.add("dve", lambda e: e.reciprocal(out, in_), r=r, w=w)


def build_program(debug=False):
    nc = bass.Bass("TRN2", target_bir_lowering=False)
    S = Sched(nc)

    def din(name, shape, dt=F32):
        return nc.dram_tensor(name, list(shape), dt, kind="ExternalInput").ap()

    x_d = din("x", [TOK, D])
    xh_d = din("xh", [128, D])
    flag_d = din("flag", [1])
    rmask_d = din("rmask", [NCORES])
    c_d = din("c", [D])
    ada_w_d = din("ada_w", [D, 1536])
    ada_b_d = din("ada_b", [6 * D])
    g_mix_d = din("norm_mix_g", [D])
    w_in_d = din("w_in", [D, IN_COLS])
    b_gates_d = din("b_gates", [8])
    conv_w_d = din("conv_w", [4, D])
    conv_b_d = din("conv_b", [D])
    gn_d = din("gmlp_norm_g", [8, 128])
    wsp_d = din("gmlp_w_spatial", [8, 128, 128])
    bsp_d = din("gmlp_b_spatial", [8 * 128])
    mn_d = din("mlstm_norm_g", [4 * 256])
    w_out_d = din("w_out", [D, D])
    g_ffn_d = din("norm_ffn_g", [D])
    wq_d = din("peer_w_query", [D, D])
    k1_d = din("peer_sub_keys_1", [128, 128])
    k2_d = din("peer_sub_keys_2", [128, 128])
    down_d = din("peer_expert_down", [16384, D])
    up_d = din("peer_expert_up", [16384, D])
    fg_d = din("final_norm_g", [D])
    y_d = nc.dram_tensor("y", [TOK, D], F32, kind="ExternalOutput").ap()

    x1_d = nc.dram_tensor("x1_s", [TOK, D], F32).ap()
    h2T_d = nc.dram_tensor("h2T_s", [16, 128, TOK], BF16).ap()
    GT_d = nc.dram_tensor("GT_s", [128, 128, TOK], BF16).ap()
    SOUT = 4 * 768 + 4
    st_in_t = nc.dram_tensor("st_in", [128, SOUT], F32)
    st_all_t = nc.dram_tensor("st_all", [NCORES * 128, SOUT], F32)
    wbf_d = nc.dram_tensor("wbf_s", [66, 128, 16 * 128], BF16).ap()
    mod_in_t = nc.dram_tensor("mod_in", [1, 1536], F32)
    mod_all_t = nc.dram_tensor("mod_all", [NCORES, 1536], F32)

    with ExitStack() as top:
        def sbT(es, name, shape, dt=F32):
            return es.enter_context(nc.sbuf_tensor(name, list(shape), dt))

        PS = [top.enter_context(nc.psum_tensor("ps%d" % i, [128, 512], F32)) for i in range(7)]
        PSB = top.enter_context(nc.psum_tensor("psb", [128, 1024], BF16))

        G = lambda n, s, d=F32: sbT(top, n, s, d)
        ident = G("ident", [128, 128])
        identb = G("identb", [128, 128], BF16)
        ones_f = G("ones_f", [128, 128])
        Umask = G("Umask", [128, 128])
        io_i = G("io_i", [128, 128], I32)
        io_pi = G("io_pi", [128, 1], I32)
        io_f = G("io_f", [128, 128])
        io_p = G("io_p", [128, 1])
        modc = G("modc", [128, 96])
        A_m = G("A_m", [128, 16])
        A_f = G("A_f", [128, 16])
        gmix_c = G("gmix_c", [128, 16])
        gffn_c = G("gffn_c", [128, 16])
        convw_c = G("convw_c", [128, 64])
        convb_c = G("convb_c", [128, 16])
        gn_c = G("gn_c", [128, 8])
        mn_c = G("mn_c", [128, 8])
        bgi_c = G("bgi_c", [4, 1])
        bgf_c = G("bgf_c", [4, 1])
        nbgf_c = G("nbgf_c", [4, 1])
        flag_c = G("flag_c", [128, 1])
        rmask_c = G("rmask_c", [128, NCORES])
        gatef_b = G("gatef_b", [128, D])
        fg_b = G("fg_b", [128, D])

        S.add("pool", lambda e: e.iota(io_i[:], [[1, 128]], base=0, channel_multiplier=0), w=["io_i"])
        S.add("pool", lambda e: e.iota(io_pi[:], [[1, 1]], base=0, channel_multiplier=1), w=["io_pi"])
        S.copy(io_f[:], io_i[:], r=["io_i"], w=["io_f"])
        S.copy(io_p[:], io_pi[:], r=["io_pi"], w=["io_p"])
        S.ts(ident[:], io_f[:], io_p[:, 0:1], None, ALU.is_equal, r=["io_f", "io_p"], w=["ident"])
        S.copy(identb[:], ident[:], r=["ident"], w=["identb"])
        S.ts(Umask[:], io_f[:], io_p[:, 0:1], None, ALU.is_ge, r=["io_f", "io_p"], w=["Umask"])
        S.memset(ones_f[:], 1.0, w=["ones_f"])

        def colload(dst, src_ap, name, **kw):
            S.dma(dst, src_ap, r=[], w=[name], chan="c_small", allow_slow_non_contiguous=True, **kw)

        rowbuf = G("rowbuf", [64, 128])

        def rowload_T(dst, src_rows_ap, nrows, name):
            S.dma(rowbuf[0:nrows, :], src_rows_ap, r=[], w=["rowbuf"], chan="c_small")
            S.tr(PS[6][:, 0:nrows], rowbuf[0:nrows, :], ident[0:nrows, 0:nrows], r=["rowbuf", "ident"], w=["ps6"])
            S.copy(dst, PS[6][:, 0:nrows], r=["ps6"], w=[name])

        rowload_T(gmix_c[:], g_mix_d.rearrange("(c p) -> c p", p=128), 16, "gmix_c")
        rowload_T(gffn_c[:], g_ffn_d.rearrange("(c p) -> c p", p=128), 16, "gffn_c")
        rowload_T(convb_c[:], conv_b_d.rearrange("(c p) -> c p", p=128), 16, "convb_c")
        rowload_T(convw_c[:], conv_w_d.rearrange("k (c p) -> (k c) p", p=128), 64, "convw_c")
        rowload_T(gn_c[:], gn_d, 8, "gn_c")
        rowload_T(mn_c[:], mn_d.rearrange("(c p) -> c p", p=128), 8, "mn_c")
        colload(bgi_c[:], b_gates_d[0:4].rearrange("(p o) -> p o", o=1), "bgi_c")
        colload(bgf_c[:], b_gates_d[4:8].rearrange("(p o) -> p o", o=1), "bgf_c")
        S.ts(nbgf_c[:], bgf_c[:], -1.0, None, ALU.mult, r=["bgf_c"], w=["nbgf_c"])
        S.dma(flag_c[:], flag_d.partition_broadcast(128), r=[], w=["flag_c"], chan="c_small")
        S.dma(rmask_c[:], rmask_d.partition_broadcast(128), r=[], w=["rmask_c"], chan="c_small")
        S.dma(fg_b[:], fg_d.partition_broadcast(128), r=[], w=["fg_b"], chan="c_small")

        with ExitStack() as p0:
            P = lambda n, s, d=F32: sbT(p0, n, s, d)
            c_c = P("c_c", [128, 16])
            sc_c = P("sc_c", [128, 16])
            modrow = P("modrow", [1, 6 * D])
            adab = P("adab", [1, 6 * D])
            one11 = P("one11", [1, 1])
            aw = [P("aw%d" % i, [128, 16 * 512]) for i in range(2)]
            rowload_T(c_c[:], c_d.rearrange("(c p) -> c p", p=128), 16, "c_c")
            S.actv(sc_c[:], c_c[:], AF.Silu, r=["c_c"], w=["sc_c"])
            S.dma(adab[:], ada_b_d.rearrange("(o n) -> o n", o=1), r=[], w=["adab"], chan="c_small")
            S.memset(one11[:], 1.0, w=["one11"])
            awv = ada_w_d.rearrange("(k p) c -> p k c", p=128)
            mpart = P("mpart", [1, 1536])
            for blk in range(3):
                s = blk % 2
                a3 = aw[s][:].rearrange("p (k c) -> p k c", k=16)
                S.dma(a3, awv[:, :, blk * 512:(blk + 1) * 512], r=[], w=["aw%d" % s], chan="c_aw%d" % s)
                pb = PS[blk % 2]
                for k in range(16):
                    S.mm(pb[0:1, :], sc_c[:, k:k + 1], a3[:, k, :], start=(k == 0), stop=(k == 15),
                         r=["sc_c", "aw%d" % s], w=["ps%d" % (blk % 2)])
                S.copy(mpart[0:1, blk * 512:(blk + 1) * 512], pb[0:1, :], r=["ps%d" % (blk % 2)], w=["mpart"])
            S.dma(mod_in_t.ap(), mpart[:], r=["mpart"], w=["mod_in"], chan="c_small")
            S.add("pool", lambda e: e.collective_compute("AllGather", ALU.bypass, replica_groups=[list(range(NCORES))],
                                                         ins=[mod_in_t.ap().opt()], outs=[mod_all_t.ap().opt()]),
                  r=["mod_in"], w=["mod_all"], chan="c_cc0", cc=True)
            S.dma(modrow[:], mod_all_t.ap().rearrange("(o r) n -> o (r n)", o=1), r=["mod_all"], w=["modrow"], chan="c_small")
            S.tt(modrow[:], modrow[:], adab[:], ALU.add, r=["modrow", "adab"], w=["modrow"])
            for j in range(96):
                S.mm(PS[2][:, j:j + 1], modrow[0:1, j * 128:(j + 1) * 128], one11[0:1, 0:1],
                     r=["modrow", "one11"], w=["ps2"])
            S.copy(modc[:], PS[2][:, 0:96], r=["ps2"], w=["modc"])
            for j in range(4):
                S.mm(PS[3][:, :], ones_f[0:1, :], modrow[0:1, 5 * D + j * 512:5 * D + (j + 1) * 512],
                     r=["ones_f", "modrow"], w=["ps3"])
                S.copy(gatef_b[:, j * 512:(j + 1) * 512], PS[3][:, :], r=["ps3"], w=["gatef_b"])
            S.stt(A_m[:], modc[:, 16:32], 1.0, gmix_c[:], ALU.add, ALU.mult, r=["modc", "gmix_c"], w=["A_m"])
            S.stt(A_f[:], modc[:, 64:80], 1.0, gffn_c[:], ALU.add, ALU.mult, r=["modc", "gffn_c"], w=["A_f"])
        S.barrier()
        shift_m = modc[:, 0:16]
        gate_m = modc[:, 32:48]
        shift_f = modc[:, 48:64]

        with ExitStack() as pc:
            P = lambda n, s, d=F32: sbT(pc, n, s, d)
            cst = [P("cst%d" % i, [128, 16 * 512]) for i in range(2)]
            cbf = [P("cbf%d" % i, [128, 16 * 512], BF16) for i in range(2)]
            w_in_v0 = w_in_d.rearrange("(k p) c -> p k c", p=128)
            w_out_v0 = w_out_d.rearrange("(k p) c -> p k c", p=128)
            blocks = [("in", b) for b in range(12)] + [("out", b) for b in range(4)]
            for bi, (which, b) in enumerate(blocks):
                s = bi % 2
                wv = w_in_v0 if which == "in" else w_out_v0
                c3 = cst[s][:].rearrange("p (k c) -> p k c", k=16)
                S.dma(c3, wv[:, :, b * 512:(b + 1) * 512], r=[], w=["cst%d" % s], chan="c_cv%d" % s)
                src4 = cst[s][:].rearrange("p (k j c) -> p j k c", k=16, j=4)
                dst4 = cbf[s][:].rearrange("p (j k c) -> p j k c", j=4, k=16)
                eng = ("dve", "act", "dve", "act", "pool")[bi % 5]
                S.copy(dst4, src4, r=["cst%d" % s], w=["cbf%d" % s], eng=eng)
                base = b * 4 if which == "in" else 50 + b * 4
                S.dma(wbf_d[base:base + 4].rearrange("j p n -> p j n"), cbf[s][:].rearrange("p (j n) -> p j n", j=4),
                      r=["cbf%d" % s], w=["wbfd%d" % (base + q) for q in range(4)], chan="c_cvo%d" % s)
            g3 = cst[0][:, 0:128].rearrange("p (k c) -> p k c", k=16)
            S.dma(g3, w_in_v0[:, :, 6144:6152], r=[], w=["cst0"], chan="c_cv0")
            for gi in range(2):
                dstg = cbf[0][:, gi * 2048:(gi + 1) * 2048].rearrange("p (k c) -> p k c", k=16)[:, :, 0:4]
                S.copy(dstg, g3[:, :, gi * 4:(gi + 1) * 4], r=["cst0"], w=["cbf0"])
            S.dma(wbf_d[48:50].rearrange("j p n -> p j n"), cbf[0][:, 0:4096].rearrange("p (j n) -> p j n", j=2),
                  r=["cbf0"], w=["wbfd48", "wbfd49"], chan="c_cvo0")
        S.barrier()

        with ExitStack() as p1:
            P = lambda n, s, d=F32: sbT(p1, n, s, d)
            xtok = [P("xtok%d" % i, [128, D]) for i in range(2)]
            xT = P("xT", [128, 16 * SN])
            hT = P("hT", [128, 16 * SN], BF16)
            yT = P("yT", [128, 16 * SN], BF16)
            NWB = 4
            wbf = [P("wbf%d" % i, [128, 16 * 128], BF16) for i in range(NWB)]
            tmpA = P("tmpA", [128, SN])
            tmpB = P("tmpB", [128, SN])
            rstd_b = P("rstd_b", [128, SN])
            raw = P("raw", [128, SN + 3])
            cacc = P("cacc", [128, SN])
            hist = P("hist", [128, 16 * 3])
            hist0 = P("hist0", [128, 16 * 3])
            qT = P("qT", [128, 2 * SN], BF16)
            kT = P("kT", [128, 2 * SN], BF16)
            vTb = P("vTb", [128, SN], BF16)
            oT = P("oT", [128, 2 * SN], BF16)
            ktok = P("ktok", [128, 4 * 256], BF16)
            vext = P("vext", [128, 4 * 384], BF16)
            iT = P("iT", [4, SN])
            lfT = P("lfT", [4, SN])
            itok = P("itok", [128, 16])
            lftok = P("lftok", [128, 16])
            CN = P("CN", [128, 4 * 768])
            CNb = P("CNb", [128, 4 * 768], BF16)
            Btot = P("Btot", [128, 4])
            lfrep = [P("lfrep%d" % i, [128, 128]) for i in range(4)]
            bcols = [P("bcols%d" % i, [128, 4]) for i in range(4)]
            argt = [P("argt%d" % i, [128, 128]) for i in range(4)]
            DT = [P("DT%d" % i, [128, 128]) for i in range(4)]
            St = [P("St%d" % i, [128, 128], BF16) for i in range(4)]
            ebb = [P("ebb%d" % i, [128, 128]) for i in range(4)]
            qw = [P("qw%d" % i, [128, 256], BF16) for i in range(4)]
            ktil = [P("ktil%d" % i, [128, 256], BF16) for i in range(4)]
            smn = [P("smn%d" % i, [128, 4]) for i in range(4)]
            smallc = P("smallc", [128, 8])
            dinv = [P("dinv%d" % i, [128, 128]) for i in range(4)]
            hm = P("hm", [128, 2 * SN])
            uT = P("uT", [128, SN], BF16)
            vg = P("vg", [128, SN])
            vnT = P("vnT", [128, SN], BF16)
            vntok = P("vntok", [128, 4 * 128], BF16)
            WspT = P("WspT", [128, 8 * 128], BF16)
            bsp_b = P("bsp_b", [128, 8 * 128])
            wsp_ld = P("wsp_ld", [128, 128])

            xT3 = xT[:].rearrange("p (k n) -> p k n", k=16)
            hT3 = hT[:].rearrange("p (k n) -> p k n", k=16)
            yT3 = yT[:].rearrange("p (k n) -> p k n", k=16)
            hist3 = hist[:].rearrange("p (c k) -> p c k", k=3)
            hist03 = hist0[:].rearrange("p (c k) -> p c k", k=3)
            convw3 = convw_c[:].rearrange("p (k c) -> p c k", k=4)
            qT3 = qT[:].rearrange("p (c n) -> p c n", c=2)
            kT3 = kT[:].rearrange("p (c n) -> p c n", c=2)
            oT3 = oT[:].rearrange("p (c n) -> p c n", c=2)
            hm3 = hm[:].rearrange("p (c n) -> p c n", c=2)
            ktok3 = ktok[:].rearrange("p (n d) -> p n d", n=4)
            vext3 = vext[:].rearrange("p (n d) -> p n d", n=4)
            CN4 = CN[:].rearrange("p (h c d) -> p h c d", h=4, c=2)
            CNb4 = CNb[:].rearrange("p (h c d) -> p h c d", h=4, c=2)
            WspT3 = WspT[:].rearrange("p (g t) -> p g t", g=8)
            bsp3 = bsp_b[:].rearrange("p (g t) -> p g t", g=8)
            vntok3 = vntok[:].rearrange("p (n c) -> p n c", n=4)
            xhT3 = hT[:, 0:16 * 128].rearrange("p (k n) -> p k n", k=16)
            stout = xT[:, 0:SOUT]
            stld = xT[:, 4096:4096 + SOUT]

            S.dma(bsp_b[:], bsp_d.partition_broadcast(128), r=[], w=["bsp_b"], chan="c_small")
            for g in range(8):
                S.dma(wsp_ld[:], wsp_d[g], r=[], w=["wsp_ld"], chan="c_small")
                S.tr(PS[2][:, 0:128], wsp_ld[:], ident[:], r=["wsp_ld", "ident"], w=["ps2"])
                S.tt(WspT3[:, g, :], PS[2][:, 0:128], Umask[:], ALU.mult, r=["ps2", "Umask"], w=["WspT"])
            S.memset(vext[:], 1.0, w=["vext"])

            w_in_v = w_in_d.rearrange("(k p) c -> p k c", p=128)
            w_out_v = w_out_d.rearrange("(k p) c -> p k c", p=128)

            wcount = [0]

            def w_load(wview, col0, ncols):
                s = wcount[0] % NWB
                wcount[0] += 1
                if wview is w_out_v:
                    idx = 50 + col0 // 128
                elif col0 == 6144:
                    idx = 48
                elif col0 == 6148:
                    idx = 49
                else:
                    idx = col0 // 128
                S.dma(wbf[s][:], wbf_d[idx], r=["wbfd%d" % idx], w=["wbf%d" % s], chan="c_w%d" % s)
                return s

            pcount = [0]

            def w_mm(s, ncols, rhs3, n, rname):
                pb = pcount[0] % 2
                pcount[0] += 1
                b3 = wbf[s][:].rearrange("p (k c) -> p k c", k=16)
                for k in range(16):
                    S.mm(PS[pb][0:ncols, 0:n], b3[:, k, 0:ncols], rhs3[:, k, 0:n], start=(k == 0), stop=(k == 15),
                         r=["wbf%d" % s, rname], w=["ps%d" % pb])
                return pb

            def run_jobs(jobs, rhs3, n, rname):
                slots = [None] * len(jobs)
                for i in range(min(NWB - 1, len(jobs))):
                    slots[i] = w_load(jobs[i][0], jobs[i][1], jobs[i][2])
                pbs = [None] * len(jobs)
                if jobs:
                    pbs[0] = w_mm(slots[0], jobs[0][2], rhs3, n, rname)
                for i, (wv, c0, ncl, epi) in enumerate(jobs):
                    nx = i + NWB - 1
                    if nx < len(jobs):
                        slots[nx] = w_load(jobs[nx][0], jobs[nx][1], jobs[nx][2])
                    if i + 1 < len(jobs):
                        pbs[i + 1] = w_mm(slots[i + 1], jobs[i + 1][2], rhs3, n, rname)
                    epi(pbs[i])

            def norm_to_h(src3, n, Acol, shcol, dst3, dname, sname):
                for k in range(16):
                    S.actv(tmpA[:, 0:n], src3[:, k, 0:n], AF.Square, r=[sname], w=["tmpA"])
                    S.mm(PS[2][:, 0:n], ones_f[:], tmpA[:, 0:n], start=(k == 0), stop=(k == 15),
                         r=["ones_f", "tmpA"], w=["ps2"])
                S.ts(rstd_b[:, 0:n], PS[2][:, 0:n], 1.0 / D, EPS, ALU.mult, op1=ALU.add, r=["ps2"], w=["rstd_b"])
                S.actv(rstd_b[:, 0:n], rstd_b[:, 0:n], AF.Sqrt, r=["rstd_b"], w=["rstd_b"])
                S.recip(rstd_b[:, 0:n], rstd_b[:, 0:n], r=["rstd_b"], w=["rstd_b"])
                for k in range(16):
                    S.tt(tmpB[:, 0:n], src3[:, k, 0:n], rstd_b[:, 0:n], ALU.mult, r=[sname, "rstd_b"], w=["tmpB"])
                    S.actv(dst3[:, k, 0:n], tmpB[:, 0:n], AF.Identity, bias=shcol[:, k:k + 1], scale=Acol[:, k:k + 1],
                           r=["tmpB", "A_m", "A_f", "modc"], w=[dname])

            def load_transpose(src_rows_ap_fn, nsub, dst3, dname):
                for sub in range(nsub):
                    s = sub % 2
                    S.dma(xtok[s][:], src_rows_ap_fn(sub), r=[], w=["xtok%d" % s], chan="c_x%d" % s)
                    for k4 in range(4):
                        pb = 3 + (k4 % 2)
                        for kk in range(4):
                            k = k4 * 4 + kk
                            S.tr(PS[pb][:, kk * 128:(kk + 1) * 128], xtok[s][:, k * 128:(k + 1) * 128], ident[:],
                                 r=["xtok%d" % s, "ident"], w=["ps%d" % pb])
                        S.copy(dst3[:, k4 * 4:(k4 + 1) * 4, sub * 128:(sub + 1) * 128],
                               PS[pb][:].rearrange("p (k n) -> p k n", k=4), r=["ps%d" % pb], w=[dname], eng="act")

            load_transpose(lambda sub: xh_d[:, :], 1, xT3, "xT")
            norm_to_h(xT3, 128, A_m, shift_m, xhT3, "hT", "xT")

            def halo_epi(c):
                def epi(pb):
                    S.ts(hist03[:, c, :], PS[pb][:, 125:128], flag_c[:, 0:1], None, ALU.mult,
                         r=["ps%d" % pb, "flag_c"], w=["hist0"])
                return epi
            run_jobs([(w_in_v, 2048 + c * 128, 128, halo_epi(c)) for c in range(16)], xhT3, 128, "hT")

            def conv_silu(pb, c, dst_ap, scale_q):
                S.copy(raw[:, 0:3], hist3[:, c, :], r=["hist"], w=["raw"])
                S.copy(raw[:, 3:3 + SN], PS[pb][:, :], r=["ps%d" % pb], w=["raw"], eng="act")
                S.copy(hist3[:, c, :], raw[:, SN:SN + 3], r=["raw"], w=["hist"])
                S.ts(cacc[:], raw[:, 0:SN], convw3[:, c, 0:1], None, ALU.mult, r=["raw", "convw_c"], w=["cacc"])
                for tap in range(1, 4):
                    S.stt(cacc[:], raw[:, tap:tap + SN], convw3[:, c, tap:tap + 1], cacc[:], ALU.mult, ALU.add,
                          r=["raw", "convw_c", "cacc"], w=["cacc"])
                if scale_q:
                    S.actv(tmpA[:], cacc[:], AF.Silu, bias=convb_c[:, c:c + 1], r=["cacc", "convb_c"], w=["tmpA"])
                    S.ts(dst_ap, tmpA[:], 0.0625, None, ALU.mult, r=["tmpA"], w=["qkdst"])
                else:
                    S.actv(dst_ap, cacc[:], AF.Silu, bias=convb_c[:, c:c + 1], r=["cacc", "convb_c"], w=["qkdst"])

            def to_tokmajor_bf(srcT_ap, dst_fn, dname, sname):
                for n in range(4):
                    S.tr(PSB[:, n * 128:(n + 1) * 128], srcT_ap[:, n * 128:(n + 1) * 128], identb[:],
                         r=[sname, "identb"], w=["psb"])
                for n in range(4):
                    S.copy(dst_fn(n), PSB[:, n * 128:(n + 1) * 128], r=["psb"], w=[dname])

            def gates_epilogues():
                def epi_i(pb):
                    S.actv(iT[:], PS[pb][0:4, :], AF.Identity, bias=bgi_c[:, 0:1], r=["ps%d" % pb, "bgi_c"], w=["iT"])
                    for n in range(4):
                        S.tr(PS[2][:, n * 4:(n + 1) * 4], iT[0:4, n * 128:(n + 1) * 128], ident[0:4, 0:4],
                             r=["iT", "ident"], w=["ps2"])
                    S.copy(itok[:], PS[2][:, 0:16], r=["ps2"], w=["itok"])

                def epi_f(pb):
                    S.actv(lfT[:], PS[pb][0:4, :], AF.Exp, bias=nbgf_c[:, 0:1], scale=-1.0,
                           r=["ps%d" % pb, "nbgf_c"], w=["lfT"])
                    S.actv(lfT[:], lfT[:], AF.Ln, bias=1.0, r=["lfT"], w=["lfT"])
                    for n in range(4):
                        S.tr(PS[2][:, n * 4:(n + 1) * 4], lfT[0:4, n * 128:(n + 1) * 128], ident[0:4, 0:4],
                             r=["lfT", "ident"], w=["ps2"])
                    S.ts(lftok[:], PS[2][:, 0:16], -1.0, None, ALU.mult, r=["ps2"], w=["lftok"])
                return epi_i, epi_f

            def mlstm_head(h, state_only):
                NN = range(4)
                cols = [n * 4 + h for n in NN]
                sls = [slice(n * 128, (n + 1) * 128) for n in NN]
                bts = [PS[2][:, 0:129], PS[2][:, 256:385], PS[3][:, 0:129], PS[3][:, 256:385]]
                btn = ["ps2", "ps2", "ps3", "ps3"]
                for n in NN:
                    S.copy(lfrep[n][:], lftok[:, cols[n]:cols[n] + 1].broadcast_to([128, 128]), r=["lftok"], w=["lfrep%d" % n])
                for n in NN:
                    S.mm(bts[n][:, 0:128], lfrep[n][:], Umask[:], r=["lfrep%d" % n, "Umask"], w=[btn[n]])
                    S.mm(bts[n][:, 128:129], Umask[:], lftok[:, cols[n]:cols[n] + 1], r=["Umask", "lftok"], w=[btn[n]])
                for n in NN:
                    S.copy(bcols[n][:, 0:1], bts[n][:, 128:129], r=[btn[n]], w=["bcols%d" % n])
                    S.copy(bcols[n][:, 1:2], bts[n][:, 127:128], r=[btn[n]], w=["bcols%d" % n])
                for n in NN:
                    S.actv(smn[n][:, 0:1], bcols[n][:, 1:2], AF.Exp, r=["bcols%d" % n], w=["smn%d" % n])
                    S.tt(smn[n][:, 1:2], bcols[n][:, 1:2], itok[:, cols[n]:cols[n] + 1], ALU.add,
                         r=["bcols%d" % n, "itok"], w=["smn%d" % n])
                for n in NN:
                    S.actv(smn[n][:, 2:3], bcols[n][:, 0:1], AF.Exp, bias=smn[n][:, 1:2], scale=-1.0,
                           r=["bcols%d" % n, "smn%d" % n], w=["smn%d" % n])
                    S.tt(Btot[:, h:h + 1], Btot[:, h:h + 1], bcols[n][:, 1:2], ALU.add, r=["Btot", "bcols%d" % n], w=["Btot"],
                         eng="pool")
                for n in NN:
                    S.ts(ktil[n][:], ktok3[:, n, :], smn[n][:, 2:3], None, ALU.mult, r=["ktok", "smn%d" % n], w=["ktil%d" % n])
                if not state_only:
                    for n in NN:
                        S.ts(argt[n][:], bts[n][:, 0:128], bcols[n][:, 0:1], 0.0, ALU.subtract, op1=ALU.min,
                             r=[btn[n], "bcols%d" % n], w=["argt%d" % n])
                        S.actv(ebb[n][:], bts[n][:, 0:128], AF.Exp, r=[btn[n]], w=["ebb%d" % n])
                    for n in NN:
                        S.actv(DT[n][:], argt[n][:], AF.Exp, bias=itok[:, cols[n]:cols[n] + 1], r=["argt%d" % n, "itok"], w=["DT%d" % n])
                        for c in range(2):
                            S.mm(PS[4][:, sls[n]], kT3[:, c, sls[n]], qT3[:, c, sls[n]], start=(c == 0), stop=(c == 1),
                                 r=["qkdst"], w=["ps4"])
                    for n in NN:
                        S.tt(DT[n][:], DT[n][:], Umask[:], ALU.mult, r=["DT%d" % n, "Umask"], w=["DT%d" % n], eng="pool")
                        for c in range(2):
                            S.tt(qw[n][:, c * 128:(c + 1) * 128], qT3[:, c, sls[n]], ebb[n][:], ALU.mult,
                                 r=["qkdst", "ebb%d" % n], w=["qw%d" % n], eng="pool")
                    for n in NN:
                        S.tt(St[n][:], PS[4][:, sls[n]], DT[n][:], ALU.mult, r=["ps4", "DT%d" % n], w=["St%d" % n])
                for n in NN:
                    if not state_only:
                        pi = 5 if n % 2 == 0 else 2
                        pnd = PS[pi]
                        pn = "ps%d" % pi
                        for j in range(3):
                            S.mm(pnd[:, j * 128:(j + 1) * 128], vext3[:, n, j * 128:(j + 1) * 128], St[n][:],
                                 start=True, stop=False, r=["vext", "St%d" % n], w=[pn])
                            for c in range(2):
                                S.mm(pnd[:, j * 128:(j + 1) * 128], CNb4[:, h, c, j * 128:(j + 1) * 128],
                                     qw[n][:, c * 128:(c + 1) * 128], start=False, stop=(c == 1),
                                     r=["CNb", "qw%d" % n], w=[pn])
                    for c in range(2):
                        ui = 6 if c == 0 else 3
                        S.mm(PS[ui][:, 0:384], ktil[n][:, c * 128:(c + 1) * 128], vext3[:, n, :], r=["ktil%d" % n, "vext"],
                             w=["ps%d" % ui])
                        S.stt(CNb4[:, h, c, :], CN4[:, h, c, :], smn[n][:, 0:1], PS[ui][:, 0:384], ALU.mult, ALU.add,
                              r=["CN", "smn%d" % n, "ps%d" % ui], w=["CNb"])
                        S.stt(CN4[:, h, c, :], CN4[:, h, c, :], smn[n][:, 0:1], PS[ui][:, 0:384], ALU.mult, ALU.add,
                              r=["CN", "smn%d" % n, "ps%d" % ui], w=["CN"])
                    if not state_only:
                        S.actv(dinv[n][:], pnd[:, 256:384], AF.Abs, r=[pn], w=["dinv%d" % n])
                        S.ts(dinv[n][:], dinv[n][:], 1.0, None, ALU.max, r=["dinv%d" % n], w=["dinv%d" % n])
                        S.recip(dinv[n][:], dinv[n][:], r=["dinv%d" % n], w=["dinv%d" % n])
                        for c in range(2):
                            S.tt(hm3[:, c, sls[n]], pnd[:, c * 128:(c + 1) * 128], dinv[n][:], ALU.mult,
                                 r=[pn, "dinv%d" % n], w=["hm"])
                if not state_only:
                    for c in range(2):
                        S.actv(tmpA[:], hm3[:, c, :], AF.Square, r=["hm"], w=["tmpA"])
                        S.mm(PS[2][:, :], ones_f[:], tmpA[:], start=(c == 0), stop=(c == 1), r=["ones_f", "tmpA"], w=["ps2"])
                    S.ts(rstd_b[:], PS[2][:, :], 1.0 / 256, EPS, ALU.mult, op1=ALU.add, r=["ps2"], w=["rstd_b"])
                    S.actv(rstd_b[:], rstd_b[:], AF.Sqrt, r=["rstd_b"], w=["rstd_b"])
                    S.recip(rstd_b[:], rstd_b[:], r=["rstd_b"], w=["rstd_b"])
                    for c in range(2):
                        S.tt(tmpB[:], hm3[:, c, :], rstd_b[:], ALU.mult, r=["hm", "rstd_b"], w=["tmpB"])
                        S.stt(yT3[:, 8 + 2 * h + c, :], tmpB[:], mn_c[:, 2 * h + c:2 * h + c + 1], oT3[:, c, :],
                              ALU.mult, ALU.mult, r=["tmpB", "mn_c", "oT"], w=["yT"])

            def mixer_pass(state_only):
                S.copy(hist[:], hist0[:], r=["hist0"], w=["hist"])
                for st in range(NST):
                    t0 = st * SN
                    load_transpose(lambda sub: x_d[t0 + sub * 128:t0 + (sub + 1) * 128, :], 4, xT3, "xT")
                    norm_to_h(xT3, SN, A_m, shift_m, hT3, "hT", "xT")
                    epi_i, epi_f = gates_epilogues()
                    jobs = [(w_in_v, 6144, 4, epi_i), (w_in_v, 6148, 4, epi_f)]
                    for h in range(4):
                        if not state_only:
                            for c in range(2):
                                def epi_q(pb, c=c, h=h):
                                    conv_silu(pb, 2 * h + c, qT3[:, c, :], True)
                                jobs.append((w_in_v, 2048 + h * 256 + c * 128, 128, epi_q))
                        for c in range(2):
                            def epi_k(pb, c=c, h=h):
                                conv_silu(pb, 8 + 2 * h + c, kT3[:, c, :], False)
                                to_tokmajor_bf(kT3[:, c, :], lambda n: ktok3[:, n, c * 128:(c + 1) * 128], "ktok", "qkdst")
                            jobs.append((w_in_v, 3072 + h * 256 + c * 128, 128, epi_k))
                        for c in range(2):
                            last_v = (c == 1)

                            def epi_v(pb, c=c, h=h, last_v=last_v):
                                S.copy(vTb[:], PS[pb][:, :], r=["ps%d" % pb], w=["vTb"], eng="act")
                                to_tokmajor_bf(vTb[:], lambda n: vext3[:, n, c * 128:(c + 1) * 128], "vext", "vTb")
                                if last_v and state_only:
                                    mlstm_head(h, True)
                            jobs.append((w_in_v, 4096 + h * 256 + c * 128, 128, epi_v))
                        if not state_only:
                            for c in range(2):
                                def epi_o(pb, c=c, h=h):
                                    S.actv(oT3[:, c, :], PS[pb][:, :], AF.Sigmoid, r=["ps%d" % pb], w=["oT"])
                                    if c == 1:
                                        mlstm_head(h, False)
                                jobs.append((w_in_v, 5120 + h * 256 + c * 128, 128, epi_o))
                    if not state_only:
                        for g in range(8):
                            def epi_u(pb, g=g):
                                S.actv(uT[:], PS[pb][:, :], AF.Gelu, r=["ps%d" % pb], w=["uT"])

                            def epi_vg(pb, g=g):
                                S.actv(vg[:], PS[pb][:, :], AF.Gelu, r=["ps%d" % pb], w=["vg"])
                                S.tt(tmpA[:], vg[:], vg[:], ALU.mult, r=["vg"], w=["tmpA"], eng="pool")
                                S.mm(PS[2][:, :], ones_f[:], tmpA[:], r=["ones_f", "tmpA"], w=["ps2"])
                                S.ts(rstd_b[:], PS[2][:, :], 1.0 / 128, EPS, ALU.mult, op1=ALU.add, r=["ps2"], w=["rstd_b"])
                                S.actv(rstd_b[:], rstd_b[:], AF.Sqrt, r=["rstd_b"], w=["rstd_b"])
                                S.recip(rstd_b[:], rstd_b[:], r=["rstd_b"], w=["rstd_b"])
                                S.stt(vnT[:], vg[:], gn_c[:, g:g + 1], rstd_b[:], ALU.mult, ALU.mult,
                                      r=["vg", "gn_c", "rstd_b"], w=["vnT"])
                                to_tokmajor_bf(vnT[:], lambda n: vntok3[:, n, :], "vntok", "vnT")
                                for n in range(4):
                                    S.mm(PS[4][:, n * 128:(n + 1) * 128], vntok3[:, n, :], WspT3[:, g, :],
                                         r=["vntok", "WspT"], w=["ps4"])
                                S.tt(tmpB[:].rearrange("p (n t) -> p n t", n=4), PS[4][:].rearrange("p (n t) -> p n t", n=4),
                                     bsp3[:, g, :].unsqueeze(1).broadcast_to([128, 4, 128]), ALU.add,
                                     r=["ps4", "bsp_b"], w=["tmpB"])
                                S.tt(yT3[:, g, :], tmpB[:], uT[:], ALU.mult, r=["tmpB", "uT"], w=["yT"])
                            jobs.append((w_in_v, g * 128, 128, epi_u))
                            jobs.append((w_in_v, 1024 + g * 128, 128, epi_vg))
                    run_jobs(jobs, hT3, SN, "hT")
                    if state_only:
                        continue
                    ojobs = []
                    for j in range(16):
                        def epi_out(pb, j=j):
                            S.stt(xT3[:, j, :], PS[pb][:, :], gate_m[:, j:j + 1], xT3[:, j, :], ALU.mult, ALU.add,
                                  r=["ps%d" % pb, "modc", "xT"], w=["xT"])
                        ojobs.append((w_out_v, j * 128, 128, epi_out))
                    run_jobs(ojobs, yT3, SN, "yT")
                    norm_to_h(xT3, SN, A_f, shift_f, hT3, "hT", "xT")
                    S.dma(h2T_d[:, :, t0:t0 + SN].rearrange("k p n -> p k n"), hT3, r=["hT"], w=["h2T_d"], chan="c_h2o")
                    for sub in range(4):
                        s = sub % 2
                        for k4 in range(4):
                            pb = 3 + (k4 % 2)
                            for kk in range(4):
                                k = k4 * 4 + kk
                                S.tr(PS[pb][:, kk * 128:(kk + 1) * 128], xT3[:, k, sub * 128:(sub + 1) * 128], ident[:],
                                     r=["xT", "ident"], w=["ps%d" % pb])
                            S.copy(xtok[s][:, k4 * 512:(k4 + 1) * 512], PS[pb][:, :], r=["ps%d" % pb], w=["xtok%d" % s], eng="act")
                        S.dma(x1_d[t0 + sub * 128:t0 + (sub + 1) * 128, :], xtok[s][:], r=["xtok%d" % s], w=["x1_d"],
                              chan="c_x1o%d" % s)

            S.memset(CN[:], 0.0, w=["CN"])
            S.memset(CNb[:], 0.0, w=["CNb"], eng="pool")
            S.memset(Btot[:], 0.0, w=["Btot"])
            mixer_pass(True)
            S.copy(stout[:, 0:4 * 768], CN[:], r=["CN"], w=["xT"])
            S.copy(stout[:, 4 * 768:SOUT], Btot[:], r=["Btot"], w=["xT"])
            S.dma(st_in_t.ap(), stout, r=["xT"], w=["st_in"], chan="c_st")
            S.add("pool", lambda e: e.collective_compute("AllGather", ALU.bypass, replica_groups=[list(range(NCORES))],
                                                         ins=[st_in_t.ap().opt()], outs=[st_all_t.ap().opt()]),
                  r=["st_in"], w=["st_all"], chan="c_cc", cc=True)
            S.memset(CN[:], 0.0, w=["CN"])
            st_all = st_all_t.ap()
            for rr in range(NCORES):
                S.dma(stld, st_all[rr * 128:(rr + 1) * 128, :], r=["st_all"], w=["xT"], chan="c_stl")
                S.actv(smallc[:, 0:4], stld[:, 4 * 768:SOUT], AF.Exp, r=["xT"], w=["smallc"])
                S.ts(smallc[:, 0:4], smallc[:, 0:4], -1.0, rmask_c[:, rr:rr + 1], ALU.add, op1=ALU.mult,
                     r=["smallc", "rmask_c"], w=["smallc"])
                S.ts(smallc[:, 0:4], smallc[:, 0:4], 1.0, None, ALU.add, r=["smallc"], w=["smallc"])
                for h in range(4):
                    S.ts(CN[:, h * 768:(h + 1) * 768], CN[:, h * 768:(h + 1) * 768], smallc[:, h:h + 1], None, ALU.mult,
                         r=["CN", "smallc"], w=["CN"])
                    S.stt(CN[:, h * 768:(h + 1) * 768], stld[:, h * 768:(h + 1) * 768], rmask_c[:, rr:rr + 1],
                          CN[:, h * 768:(h + 1) * 768], ALU.mult, ALU.add, r=["CN", "xT", "rmask_c"], w=["CN"])
            S.copy(CNb[:], CN[:], r=["CN"], w=["CNb"])
            S.memset(Btot[:], 0.0, w=["Btot"])
            mixer_pass(False)

        S.barrier()
        with ExitStack() as p2:
            P = lambda n, s, d=F32: sbT(p2, n, s, d)
            wq = P("wq", [128, 16 * D], BF16)
            K1T = P("K1T", [128, 128])
            K2T = P("K2T", [128, 128])
            kld = P("kld", [128, 128])
            h2l = [P("h2l%d" % i, [128, 16 * 128], BF16) for i in range(2)]
            qTf = P("qTf", [128, 16 * 128])
            Sc = P("Sc", [128, 16 * 128])
            Sc_b = P("Sc_b", [128, 16 * 128])
            v16 = P("v16", [128, 16 * 16])
            wk = P("wk", [128, 256])
            cand = P("cand", [128, 8 * 256])
            m8 = P("m8", [128, 16])
            ec = P("ec", [128, 256])
            ecm = P("ecm", [128, 256])
            tau = P("tau", [128, 8])
            tau_b = P("tau_b", [128, 8])
            mx = P("mx", [128, 8])
            Zs = P("Zs", [128, 8])
            nb = P("nb", [128, 8])
            nb_b = P("nb_b", [128, 8])
            AB = 8
            wqst = [P("wqst%d" % i, [128, 16 * 128]) for i in range(2)]
            zb = [P("zb%d" % i, [128, AB * 128]) for i in range(3)]
            eb = [P("eb%d" % i, [128, AB * 128], BF16) for i in range(3)]
            tm = [P("tm%d" % i, [128, AB * 128], BF16) for i in range(3)]
            GTs = [P("GTs%d" % i, [128, AB * 128], BF16) for i in range(2)]

            wq3 = wq[:].rearrange("p (k c) -> p k c", k=16)
            wq_v = wq_d.rearrange("(k p) c -> p k c", p=128)
            wqs = wqst
            for j in range(16):
                s = j % 2
                w3 = wqs[s][:].rearrange("p (k c) -> p k c", k=16)
                S.dma(w3, wq_v[:, :, j * 128:(j + 1) * 128], r=[], w=["wqst%d" % s], chan="c_wq%d" % s)
                S.copy(wq3[:, :, j * 128:(j + 1) * 128], w3, r=["wqst%d" % s], w=["wq"], eng="pool")
            for (kd, KT, nm) in ((k1_d, K1T, "K1T"), (k2_d, K2T, "K2T")):
                S.dma(kld[:], kd, r=[], w=["kld"], chan="c_small")
                S.tr(PS[2][:, 0:128], kld[:], ident[:], r=["kld", "ident"], w=["ps2"])
                S.copy(KT[:], PS[2][:, 0:128], r=["ps2"], w=[nm])

            qTf3 = qTf[:].rearrange("p (j n) -> p j n", j=16)
            Sc3 = Sc[:].rearrange("p (j n) -> p j n", j=16)
            v163 = v16[:].rearrange("p (j n) -> p j n", j=16)
            cand3 = cand[:].rearrange("p (h n) -> p h n", h=8)

            Scs = [Sc, Sc_b]
            taus = [tau, tau_b]
            nbs = [nb, nb_b]

            def small_phase(t):
                tk0 = t * 128
                q2 = t % 2
                Sc3q = Scs[q2][:].rearrange("p (j n) -> p j n", j=16)
                tauq = taus[q2]
                nbq = nbs[q2]
                ScN, tauN, nbN = "Sc%d" % q2, "tau%d" % q2, "nb%d" % q2
                s = t % 2
                h3 = h2l[s][:].rearrange("p (k n) -> p k n", k=16)
                S.dma(h3, h2T_d[:, :, tk0:tk0 + 128].rearrange("k p n -> p k n"), r=["h2T_d"], w=["h2l%d" % s], chan="c_h2l%d" % s)
                for j4 in range(4):
                    pb = j4 % 2
                    for jj in range(4):
                        j = j4 * 4 + jj
                        for k in range(16):
                            S.mm(PS[pb][:, jj * 128:(jj + 1) * 128], wq3[:, k, j * 128:(j + 1) * 128], h3[:, k, :],
                                 start=(k == 0), stop=(k == 15), r=["wq", "h2l%d" % s], w=["ps%d" % pb])
                    S.copy(qTf3[:, j4 * 4:(j4 + 1) * 4, :], PS[pb][:].rearrange("p (j n) -> p j n", j=4),
                           r=["ps%d" % pb], w=["qTf"], eng="act")
                for j4 in range(4):
                    pb = 6 if j4 % 2 == 0 else 0
                    for jj in range(4):
                        j = j4 * 4 + jj
                        KT = K1T if j % 2 == 0 else K2T
                        S.mm(PS[pb][:, jj * 128:(jj + 1) * 128], qTf3[:, j, :], KT[:], r=["qTf", "K1T", "K2T"], w=["ps%d" % pb])
                    S.copy(Sc3q[:, j4 * 4:(j4 + 1) * 4, :], PS[pb][:].rearrange("p (j n) -> p j n", j=4),
                           r=["ps%d" % pb], w=[ScN], eng="act")
                for j in range(16):
                    S.add("dve", lambda e, j=j: e.max(out=v163[:, j, 0:8], in_=Sc3q[:, j, :]), r=[ScN], w=["v16"])
                    S.add("dve", lambda e, j=j: e.match_replace(out=wk[:, 0:128], in_to_replace=v163[:, j, 0:8],
                                                               in_values=Sc3q[:, j, :], imm_value=-1e30),
                          r=[ScN, "v16"], w=["wk"])
                    S.add("dve", lambda e, j=j: e.max(out=v163[:, j, 8:16], in_=wk[:, 0:128]), r=["wk"], w=["v16"])
                v4 = v16[:].rearrange("p (h two n) -> p h two n", h=8, two=2)
                S.tt(cand[:].rearrange("p (h a b) -> p h a b", h=8, a=16),
                     v4[:, :, 0, :].unsqueeze(3).broadcast_to([128, 8, 16, 16]),
                     v4[:, :, 1, :].unsqueeze(2).broadcast_to([128, 8, 16, 16]), ALU.add, r=["v16"], w=["cand"])
                S.tt(mx[:], v4[:, :, 0, 0], v4[:, :, 1, 0], ALU.add, r=["v16"], w=["mx"])
                S.ts(nbq[:], mx[:], -1.0, None, ALU.mult, r=["mx"], w=[nbN])
                for h in range(8):
                    S.add("dve", lambda e, h=h: e.max(out=m8[:, 0:8], in_=cand3[:, h, :]), r=["cand"], w=["m8"])
                    S.add("dve", lambda e, h=h: e.match_replace(out=wk[:], in_to_replace=m8[:, 0:8], in_values=cand3[:, h, :],
                                                               imm_value=-1e30), r=["cand", "m8"], w=["wk"])
                    S.add("dve", lambda e, h=h: e.max(out=m8[:, 8:16], in_=wk[:]), r=["wk"], w=["m8"])
                    S.copy(tauq[:, h:h + 1], m8[:, 15:16], r=["m8"], w=[tauN])
                    S.actv(ec[:], cand3[:, h, :], AF.Exp, bias=nbq[:, h:h + 1], r=["cand", nbN], w=["ec"])
                    S.stt(ecm[:], cand3[:, h, :], tauq[:, h:h + 1], ec[:], ALU.is_ge, ALU.mult, accum_out=Zs[:, h:h + 1],
                          r=["cand", tauN, "ec"], w=["ecm", "Zs"])
                S.actv(Zs[:], Zs[:], AF.Ln, r=["Zs"], w=["Zs"])
                S.tt(nbq[:], nbq[:], Zs[:], ALU.subtract, r=[nbN, "Zs"], w=[nbN])

            def dense_phase(t):
                tk0 = t * 128
                q2 = t % 2
                Sc3q = Scs[q2][:].rearrange("p (j n) -> p j n", j=16)
                tauq = taus[q2]
                nbq = nbs[q2]
                ScN, tauN, nbN = "Sc%d" % q2, "tau%d" % q2, "nb%d" % q2
                for ab in range(128 // AB):
                    bset = 2 + 2 * (ab % 2)
                    for h in range(8):
                        u = ab * 8 + h
                        i3 = u % 3
                        engA = "pool"
                        z3 = zb[i3][:].rearrange("p (a b) -> p a b", a=AB)
                        S.tt(z3, Sc3q[:, 2 * h, ab * AB:(ab + 1) * AB].unsqueeze(2).broadcast_to([128, AB, 128]),
                             Sc3q[:, 2 * h + 1, :].unsqueeze(1).broadcast_to([128, AB, 128]), ALU.add,
                             r=[ScN], w=["zb%d" % i3], eng=engA)
                        S.actv(eb[i3][:], zb[i3][:], AF.Exp, bias=nbq[:, h:h + 1], r=["zb%d" % i3, nbN], w=["eb%d" % i3])
                        S.stt(tm[i3][:], zb[i3][:], tauq[:, h:h + 1], eb[i3][:], ALU.is_ge, ALU.mult,
                              r=["zb%d" % i3, tauN, "eb%d" % i3], w=["tm%d" % i3])
                        for aa in range(AB):
                            pb = bset + aa // 4
                            S.mm(PS[pb][:, (aa % 4) * 128:(aa % 4 + 1) * 128], tm[i3][:, aa * 128:(aa + 1) * 128], identb[:],
                                 start=(h == 0 and aa % 4 == 0), stop=(h == 7), skip=True,
                                 r=["tm%d" % i3, "identb"], w=["ps%d" % pb])
                    gi = ab % 2
                    g3 = GTs[gi][:].rearrange("p (a n) -> p a n", a=AB)
                    for q4 in range(AB // 4):
                        pb = bset + q4
                        S.copy(g3[:, q4 * 4:(q4 + 1) * 4, :], PS[pb][:].rearrange("p (a n) -> p a n", a=4),
                               r=["ps%d" % pb], w=["GTs%d" % gi], eng="act")
                    S.dma(GT_d[ab * AB:(ab + 1) * AB, :, tk0:tk0 + 128].rearrange("a b n -> b a n"), g3,
                          r=["GTs%d" % gi], w=["GT_d"], chan="c_gto%d" % gi)


            def capture(fn):
                cap = []
                S.add = lambda *a_, **k_: cap.append((a_, k_))
                try:
                    fn()
                finally:
                    del S.add
                return cap

            for (a_, k_) in capture(lambda: small_phase(0)):
                S.add(*a_, **k_)
            for t in range(16):
                Dl = capture(lambda: dense_phase(t))
                Sl = capture(lambda: small_phase(t + 1)) if t + 1 < 16 else []
                si = 0
                for i_, (a_, k_) in enumerate(Dl):
                    S.add(*a_, **k_)
                    tgt = (i_ + 1) * len(Sl) // len(Dl)
                    while si < tgt:
                        S.add(*Sl[si][0], **Sl[si][1])
                        si += 1

        S.barrier()
        with ExitStack() as p3:
            P = lambda n, s, d=F32: sbT(p3, n, s, d)
            TP = 1024
            EG = 4
            NCH = 128
            acc = P("acc", [128, 8 * D])
            h2p = P("h2p", [128, 16 * TP], BF16)
            dn = [P("dn%d" % i, [128, D]) for i in range(2)]
            upl = [P("upl%d" % i, [128, D]) for i in range(2)]
            gtl = [P("gtl%d" % i, [128, TP], BF16) for i in range(2)]
            dnb = [P("dnb%d" % i, [128, D], BF16) for i in range(2)]
            dnT = [P("dnT%d" % i, [128, 16 * 128], BF16) for i in range(3)]
            upb = [P("upb%d" % i, [128, D], BF16) for i in range(EG)]
            ga = P("ga", [128, TP], BF16)
            At = [P("At%d" % i, [128, TP], BF16) for i in range(EG)]
            ssq = P("ssq", [128, 8])
            acc3 = acc[:].rearrange("p (i d) -> p i d", i=8)
            h2p3 = h2p[:].rearrange("p (k n) -> p k n", k=16)
            ocnt = [0]
            for ps_ in range(2):
                tp0 = ps_ * TP
                S.memset(acc[:, 0:4 * D], 0.0, w=["acc"])
                S.memset(acc[:, 4 * D:8 * D], 0.0, w=["acc"], eng="pool")
                S.dma(h2p3, h2T_d[:, :, tp0:tp0 + TP].rearrange("k p n -> p k n"), r=["h2T_d"], w=["h2p"], chan="c_h2p")

                def load_dn(a):
                    s = a % 2
                    S.dma(dn[s][:], down_d[a * 128:(a + 1) * 128, :], r=[], w=["dn%d" % s], chan="c_dn%d" % s)

                def load_up(a):
                    s = a % 2
                    S.dma(upl[s][:], up_d[a * 128:(a + 1) * 128, :], r=[], w=["upl%d" % s], chan="c_up%d" % s)

                def load_gt(a):
                    s = a % 2
                    S.dma(gtl[s][:], GT_d[a, :, tp0:tp0 + TP], r=["GT_d"], w=["gtl%d" % s], chan="c_gt%d" % s)

                def stage_Th(a, half):
                    s = a % 2
                    t3 = a % 3
                    if half == 0:
                        S.copy(dnb[s][:], dn[s][:], r=["dn%d" % s], w=["dnb%d" % s], eng="pool")
                    for kk in range(8):
                        k = half * 8 + kk
                        S.tr(PSB[:, kk * 128:(kk + 1) * 128], dnb[s][:, k * 128:(k + 1) * 128], identb[:],
                             r=["dnb%d" % s, "identb"], w=["psb"])
                    S.copy(dnT[t3][:, half * 1024:(half + 1) * 1024], PSB[:, :], r=["psb"], w=["dnT%d" % t3], eng="act")

                def stage_T(a):
                    stage_Th(a, 0)
                    stage_Th(a, 1)

                def stage_U(a):
                    S.copy(upb[a % EG][:], upl[a % 2][:], r=["upl%d" % (a % 2)], w=["upb%d" % (a % EG)], eng="act")

                def stage_Ah(a, hf):
                    d3 = dnT[a % 3][:].rearrange("p (k e) -> p k e", k=16)
                    pb = hf
                    for k in range(16):
                        S.mm(PS[pb][:, :], d3[:, k, :], h2p3[:, k, hf * 512:(hf + 1) * 512], start=(k == 0), stop=(k == 15),
                             r=["dnT%d" % (a % 3), "h2p"], w=["ps%d" % pb])
                    S.actv(ga[:, hf * 512:(hf + 1) * 512], PS[pb][:, :], AF.Gelu, r=["ps%d" % pb], w=["ga"])

                def stage_Afin(a):
                    s = a % 2
                    S.tt(At[a % EG][:], ga[:], gtl[s][:], ALU.mult, r=["ga", "gtl%d" % s], w=["At%d" % (a % EG)])

                def stage_O(g):
                    for i in range(8):
                        for j in range(4):
                            pb = 2 + (ocnt[0] % 5)
                            ocnt[0] += 1
                            for c in range(EG):
                                S.mm(PS[pb][:, :], At[c][:, i * 128:(i + 1) * 128], upb[c][:, j * 512:(j + 1) * 512],
                                     start=(c == 0), stop=(c == EG - 1), r=["At%d" % c, "upb%d" % c], w=["ps%d" % pb])
                            S.tt(acc3[:, i, j * 512:(j + 1) * 512], acc3[:, i, j * 512:(j + 1) * 512], PS[pb][:, :], ALU.add,
                                 r=["acc", "ps%d" % pb], w=["acc"])

                load_dn(0)
                load_dn(1)
                load_up(0)
                load_gt(0)
                stage_T(0)
                stage_T(1)
                load_dn(2)
                for a in range(NCH):
                    if a + 3 < NCH:
                        load_dn(a + 3)
                    if a + 1 < NCH:
                        load_up(a + 1)
                        load_gt(a + 1)
                    stage_U(a)
                    if a + 2 < NCH:
                        stage_Th(a + 2, 0)
                    stage_Ah(a, 0)
                    if a + 2 < NCH:
                        stage_Th(a + 2, 1)
                    stage_Ah(a, 1)
                    stage_Afin(a)
                    if a % EG == EG - 1:
                        stage_O(a // EG)
                for i in range(8):
                    s = i % 2
                    r0 = tp0 + i * 128
                    S.dma(dn[s][:], x1_d[r0:r0 + 128, :], r=["x1_d"], w=["dn%d" % s], chan="c_dn%d" % s)
                    S.tt(acc3[:, i, :], acc3[:, i, :], gatef_b[:], ALU.mult, r=["acc", "gatef_b"], w=["acc"])
                    S.tt(acc3[:, i, :], acc3[:, i, :], dn[s][:], ALU.add, r=["acc", "dn%d" % s], w=["acc"], eng="pool")
                    S.actv(dnb[s][:], acc3[:, i, :], AF.Square, accum_out=ssq[:, i:i + 1], r=["acc"], w=["dnb%d" % s, "ssq"])
                S.ts(ssq[:], ssq[:], 1.0 / D, EPS, ALU.mult, op1=ALU.add, r=["ssq"], w=["ssq"])
                S.actv(ssq[:], ssq[:], AF.Sqrt, r=["ssq"], w=["ssq"])
                S.recip(ssq[:], ssq[:], r=["ssq"], w=["ssq"])
                for i in range(8):
                    r0 = tp0 + i * 128
                    S.stt(acc3[:, i, :], acc3[:, i, :], ssq[:, i:i + 1], fg_b[:], ALU.mult, ALU.mult,
                          r=["acc", "ssq", "fg_b"], w=["acc"])
                    S.dma(y_d[r0:r0 + 128, :], acc3[:, i, :], r=["acc"], w=["y"], chan="c_yo%d" % (i % 2))

        with nc.Block() as block:
            S.emit(block)
    return nc


_NC_CACHE = {}


def kernel(**inputs):
    f32 = lambda a: np.ascontiguousarray(np.asarray(a, dtype=np.float32))
    x = f32(inputs["x"])[0]
    shared = {
        "c": f32(inputs["c"])[0],
        "ada_b": f32(inputs["ada_b"])[0],
        "norm_mix_g": f32(inputs["norm_mix_g"])[0],
        "w_in": f32(inputs["w_in"])[0],
        "b_gates": f32(inputs["b_gates"])[0],
        "conv_w": f32(inputs["conv_w"])[0],
        "conv_b": f32(inputs["conv_b"])[0],
        "gmlp_norm_g": f32(inputs["gmlp_norm_g"])[0],
        "gmlp_w_spatial": f32(inputs["gmlp_w_spatial"])[0],
        "gmlp_b_spatial": f32(inputs["gmlp_b_spatial"])[0].reshape(-1),
        "mlstm_norm_g": f32(inputs["mlstm_norm_g"])[0].reshape(-1),
        "w_out": f32(inputs["w_out"])[0],
        "norm_ffn_g": f32(inputs["norm_ffn_g"])[0],
        "peer_w_query": f32(inputs["peer_w_query"])[0],
        "peer_sub_keys_1": f32(inputs["peer_sub_keys_1"])[0],
        "peer_sub_keys_2": f32(inputs["peer_sub_keys_2"])[0],
        "peer_expert_down": f32(inputs["peer_expert_down"])[0],
        "peer_expert_up": f32(inputs["peer_expert_up"])[0],
        "final_norm_g": f32(inputs["final_norm_g"]),
    }
    ada_w_full = f32(inputs["ada_w"])[0]
    in_maps = []
    for r in range(NCORES):
        m = dict(shared)
        m["x"] = np.ascontiguousarray(x[r * TOK:(r + 1) * TOK])
        m["ada_w"] = np.ascontiguousarray(ada_w_full[:, r * 1536:(r + 1) * 1536])
        if r == 0:
            m["xh"] = np.zeros((128, D), np.float32)
            m["flag"] = np.zeros((1,), np.float32)
        else:
            m["xh"] = np.ascontiguousarray(x[r * TOK - 128:r * TOK])
            m["flag"] = np.ones((1,), np.float32)
        rm = np.zeros((NCORES,), np.float32)
        rm[:r] = 1.0
        m["rmask"] = rm
        in_maps.append(m)
    if "nc" not in _NC_CACHE:
        _NC_CACHE["nc"] = build_program()
    res = run_bass_kernel_spmd(_NC_CACHE["nc"], in_maps, core_ids=list(range(NCORES)))
    out = np.concatenate([np.asarray(res.results[r]["y"], dtype=np.float32) for r in range(NCORES)], axis=0)
    return out.reshape(1, NCORES * TOK, D)
```
